# Optimizing a Trainium2 kernel written in Bass

```python
import math
import numpy as np
import jax, jax.numpy as jnp
from jax import lax

D_MODEL = 2048
BATCH = 4
SEQ = 2048
DEPTH = 4

N_MIXERS = 2
NSA_HEADS = 16
NSA_KV_GROUPS = 2
NSA_HEAD_DIM = D_MODEL // NSA_HEADS
NSA_Q_PER_GROUP = NSA_HEADS // NSA_KV_GROUPS
CMP_BLOCK = 32
CMP_STRIDE = 16
SLC_BLOCK = 64
N_SELECT = 8
WINDOW = 512
Q_BLOCK = 128
ROPE_THETA = 500000.0
ROPE_DIM = NSA_HEAD_DIM // 4
NSA_IN_DIM = NSA_HEADS * NSA_HEAD_DIM + 3 * 2 * NSA_KV_GROUPS * NSA_HEAD_DIM + 3 * NSA_HEADS
MLSTM_HEADS = 8
MLSTM_QK_DIM = D_MODEL // (2 * MLSTM_HEADS)
MLSTM_V_DIM = D_MODEL // MLSTM_HEADS
MLSTM_CHUNK = 64
GATE_SOFTCAP = 15.0
MLSTM_IN_DIM = 2 * MLSTM_HEADS * MLSTM_QK_DIM + 2 * MLSTM_HEADS * MLSTM_V_DIM + 2 * MLSTM_HEADS
D_FF = 5632
CONV_WIDTH = 3
NORM_EPS = 1e-6
NEG_INF = -1e30

kernel_name = "nsa_mlstm_interleaved_hybrid"


def rms_norm(x, g):
    xf = x.astype(jnp.float32)
    y = xf * lax.rsqrt(jnp.mean(xf * xf, axis=-1, keepdims=True) + NORM_EPS)
    return (y * g).astype(x.dtype)


def partial_rope(x, positions):
    half = ROPE_DIM // 2
    inv = jnp.power(ROPE_THETA, -jnp.arange(0, ROPE_DIM, 2, dtype=jnp.float32) / ROPE_DIM)
    ang = positions.astype(jnp.float32)[..., None] * inv
    cos, sin = jnp.cos(ang)[:, :, None, :], jnp.sin(ang)[:, :, None, :]
    xr = x[..., :ROPE_DIM].astype(jnp.float32)
    x1, x2 = xr[..., :half], xr[..., half:]
    rot = jnp.concatenate([x1 * cos - x2 * sin, x2 * cos + x1 * sin], axis=-1)
    return jnp.concatenate([rot.astype(x.dtype), x[..., ROPE_DIM:]], axis=-1)


def gather_blocks(blocks, idx):
    return jax.vmap(jax.vmap(lambda kb, ix: kb[ix]))(blocks, idx)


def nsa_mixer(xn, positions, w_in, gate_b, cmp_pe, cmp_w1, cmp_w2, w_out):
    B, S, _ = xn.shape
    H, G, R, hd = NSA_HEADS, NSA_KV_GROUPS, NSA_Q_PER_GROUP, NSA_HEAD_DIM
    scale = hd ** -0.5
    proj = xn @ w_in
    o1 = H * hd
    o2 = o1 + 2 * G * hd
    o3 = o2 + 2 * G * hd
    o4 = o3 + 2 * G * hd
    q = proj[..., :o1].reshape(B, S, H, hd)
    kv_c = proj[..., o1:o2].reshape(B, S, 2, G, hd)
    kv_s = proj[..., o2:o3].reshape(B, S, 2, G, hd)
    kv_w = proj[..., o3:o4].reshape(B, S, 2, G, hd)
    gates = jax.nn.sigmoid(proj[..., o4:].reshape(B, S, 3, H) + gate_b)
    t = np.arange(S)

    n_cmp = (S - CMP_BLOCK) // CMP_STRIDE + 1
    cmp_idx = np.arange(n_cmp)[:, None] * CMP_STRIDE + np.arange(CMP_BLOCK)[None, :]
    blocks = kv_c[:, cmp_idx] + cmp_pe.transpose(1, 0, 2)[:, :, None, :]
    hid = jax.nn.gelu(jnp.einsum('bjlcgd,cldh->bjcgh', blocks, cmp_w1))
    kv_cmp = jnp.einsum('bjcgh,che->bjcge', hid, cmp_w2)
    k_cmp, v_cmp = kv_cmp[:, :, 0], kv_cmp[:, :, 1]
    qg = q.reshape(B, S, G, R, hd)
    cmp_mask = cmp_idx[:, -1][None, :] <= t[:, None]
    s_c = jnp.einsum('bsgrd,bjgd->bgrsj', qg, k_cmp, preferred_element_type=jnp.float32) * scale
    p_c = jax.nn.softmax(jnp.where(cmp_mask, s_c, NEG_INF), axis=-1) * cmp_mask
    o_cmp = jnp.einsum('bgrsj,bjgd->bsgrd', p_c.astype(v_cmp.dtype), v_cmp).reshape(B, S, H, hd)

    n_slc = S // SLC_BLOCK
    jj = np.arange(n_cmp)[:, None]
    ss = np.arange(n_slc)[None, :]
    lo = np.maximum(jj * CMP_STRIDE, ss * SLC_BLOCK)
    hi = np.minimum(jj * CMP_STRIDE + CMP_BLOCK, ss * SLC_BLOCK + SLC_BLOCK)
    overlap = jnp.asarray(np.clip(hi - lo, 0, None).astype(np.float32) / CMP_BLOCK)
    imp = jnp.einsum('bgrsj,jn->bgsn', p_c, overlap)
    q_blk_id = t // SLC_BLOCK
    sid = np.arange(n_slc)
    forced = (sid[None, :] == 0) | (sid[None, :] == q_blk_id[:, None])
    valid = sid[None, :] <= q_blk_id[:, None]
    score = jnp.where(forced, jnp.inf, jnp.where(valid, imp, -jnp.inf))
    n_sel = min(N_SELECT, n_slc)
    _, sel_idx = lax.top_k(score, n_sel)

    q_r = partial_rope(q, positions).reshape(B, S, G, R, hd)
    k_s = partial_rope(kv_s[:, :, 0], positions).transpose(0, 2, 1, 3)
    v_s = kv_s[:, :, 1].transpose(0, 2, 1, 3)
    ks_blocks = k_s.reshape(B, G, n_slc, SLC_BLOCK, hd)
    vs_blocks = v_s.reshape(B, G, n_slc, SLC_BLOCK, hd)
    pad = ((0, 0), (0, 0), (WINDOW, 0), (0, 0))
    kw_pad = jnp.pad(partial_rope(kv_w[:, :, 0], positions).transpose(0, 2, 1, 3), pad)
    vw_pad = jnp.pad(kv_w[:, :, 1].transpose(0, 2, 1, 3), pad)

    nqb = S // Q_BLOCK
    q_blocks = q_r.reshape(B, nqb, Q_BLOCK, G, R, hd).transpose(1, 0, 3, 4, 2, 5)
    idx_blocks = sel_idx.reshape(B, G, nqb, Q_BLOCK, n_sel).transpose(2, 0, 1, 3, 4)

    def block_step(args):
        q_b, idx_b, qb = args
        tq = qb * Q_BLOCK + jnp.arange(Q_BLOCK)
        k_g = gather_blocks(ks_blocks, idx_b).reshape(B, G, Q_BLOCK, n_sel * SLC_BLOCK, hd)
        v_g = gather_blocks(vs_blocks, idx_b).reshape(B, G, Q_BLOCK, n_sel * SLC_BLOCK, hd)
        kpos = (idx_b[..., None] * SLC_BLOCK + jnp.arange(SLC_BLOCK)).reshape(B, G, Q_BLOCK, n_sel * SLC_BLOCK)
        m_s = (kpos <= tq[:, None])[:, :, None]
        s_s = jnp.einsum('bgrqd,bgqkd->bgrqk', q_b, k_g, preferred_element_type=jnp.float32) * scale
        p_s = jax.nn.softmax(jnp.where(m_s, s_s, NEG_INF), axis=-1)
        o_s = jnp.einsum('bgrqk,bgqkd->bgrqd', p_s.astype(v_g.dtype), v_g)
        kw = lax.dynamic_slice_in_dim(kw_pad, qb * Q_BLOCK, Q_BLOCK + WINDOW, axis=2)
        vw = lax.dynamic_slice_in_dim(vw_pad, qb * Q_BLOCK, Q_BLOCK + WINDOW, axis=2)
        wpos = qb * Q_BLOCK - WINDOW + jnp.arange(Q_BLOCK + WINDOW)
        dist = tq[:, None] - wpos[None, :]
        m_w = (wpos[None, :] >= 0) & (dist >= 0) & (dist < WINDOW)
        s_w = jnp.einsum('bgrqd,bgkd->bgrqk', q_b, kw, preferred_element_type=jnp.float32) * scale
        p_w = jax.nn.softmax(jnp.where(m_w, s_w, NEG_INF), axis=-1)
        o_w = jnp.einsum('bgrqk,bgkd->bgrqd', p_w.astype(vw.dtype), vw)
        return o_s, o_w

    o_sel, o_win = lax.map(block_step, (q_blocks, idx_blocks, jnp.arange(nqb)))
    o_sel = o_sel.transpose(1, 0, 4, 2, 3, 5).reshape(B, S, H, hd)
    o_win = o_win.transpose(1, 0, 4, 2, 3, 5).reshape(B, S, H, hd)
    o = (gates[:, :, 0, :, None] * o_cmp + gates[:, :, 1, :, None] * o_sel
         + gates[:, :, 2, :, None] * o_win)
    return o.reshape(B, S, H * hd) @ w_out


def mlstm_chunkwise(q, k, v, li, lf):
    B, H, S, dk = q.shape
    dv = v.shape[-1]
    L = MLSTM_CHUNK
    nc = S // L
    qc = q.reshape(B, H, nc, L, dk)
    kc = k.reshape(B, H, nc, L, dk)
    vc = v.reshape(B, H, nc, L, dv)
    lic = li.reshape(B, H, nc, L)
    F = jnp.cumsum(lf.reshape(B, H, nc, L), axis=-1)
    F_end = F[..., -1]
    a = F_end[..., None] - F + lic
    a_max = a.max(axis=-1)
    w = jnp.exp(a - a_max[..., None])
    dC = jnp.einsum('bhcl,bhclk,bhclv->bhckv', w, kc, vc)
    dn = jnp.einsum('bhcl,bhclk->bhck', w, kc)

    def step(carry, inp):
        C, n, m = carry
        f_end, am, dC_c, dn_c = inp
        m_new = jnp.maximum(f_end + m, am)
        decay = jnp.exp(f_end + m - m_new)
        inject = jnp.exp(am - m_new)
        C_new = decay[..., None, None] * C + inject[..., None, None] * dC_c
        n_new = decay[..., None] * n + inject[..., None] * dn_c
        return (C_new, n_new, m_new), (C, n, m)

    init = (jnp.zeros((B, H, dk, dv), jnp.float32), jnp.zeros((B, H, dk), jnp.float32),
            jnp.zeros((B, H), jnp.float32))
    xs = (F_end.transpose(2, 0, 1), a_max.transpose(2, 0, 1),
          dC.transpose(2, 0, 1, 3, 4), dn.transpose(2, 0, 1, 3))
    _, (C0, n0, m0) = lax.scan(step, init, xs)
    C0 = C0.transpose(1, 2, 0, 3, 4)
    n0 = n0.transpose(1, 2, 0, 3)
    m0 = m0.transpose(1, 2, 0)

    causal = np.tril(np.ones((L, L), dtype=bool))
    D = jnp.where(causal, F[..., :, None] - F[..., None, :] + lic[..., None, :], -jnp.inf)
    inter = F + m0[..., None]
    m_t = jnp.maximum(inter, D.max(axis=-1))
    Dw = jnp.exp(D - m_t[..., None])
    g_inter = jnp.exp(inter - m_t)
    Sqk = jnp.einsum('bhclk,bhcsk->bhcls', qc, kc) * Dw
    num = (jnp.einsum('bhcls,bhcsv->bhclv', Sqk, vc)
           + g_inter[..., None] * jnp.einsum('bhclk,bhckv->bhclv', qc, C0))
    den = Sqk.sum(axis=-1) + g_inter * jnp.einsum('bhclk,bhck->bhcl', qc, n0)
    h = num / jnp.maximum(jnp.abs(den), jnp.exp(-m_t))[..., None]
    return h.reshape(B, H, S, dv)


def mlstm_mixer(xn, w_in, gate_b, head_norm, w_out):
    B, S, _ = xn.shape
    H, dk, dv = MLSTM_HEADS, MLSTM_QK_DIM, MLSTM_V_DIM
    proj = xn @ w_in
    o1 = H * dk
    o2 = o1 + H * dk
    o3 = o2 + H * dv
    o4 = o3 + H * dv
    o5 = o4 + H
    to_heads = lambda z, d: z.reshape(B, S, H, d).transpose(0, 2, 1, 3).astype(jnp.float32)
    q = to_heads(proj[..., :o1], dk) * (dk ** -0.5)
    k = to_heads(proj[..., o1:o2], dk)
    v = to_heads(proj[..., o2:o3], dv)
    og = proj[..., o3:o4]
    i_pre = (proj[..., o4:o5] + gate_b[0]).astype(jnp.float32)
    f_pre = (proj[..., o5:] + gate_b[1]).astype(jnp.float32)
    li = (GATE_SOFTCAP * jnp.tanh(i_pre / GATE_SOFTCAP)).transpose(0, 2, 1)
    lf = jax.nn.log_sigmoid(GATE_SOFTCAP * jnp.tanh(f_pre / GATE_SOFTCAP)).transpose(0, 2, 1)
    h = mlstm_chunkwise(q, k, v, li, lf)
    h = h * lax.rsqrt(jnp.mean(h * h, axis=-1, keepdims=True) + NORM_EPS) * head_norm[:, None, :]
    h = h.transpose(0, 2, 1, 3).reshape(B, S, H * dv).astype(xn.dtype) * jax.nn.sigmoid(og)
    return h @ w_out


def conv_glu_ffn(xn, w_up, conv_w, conv_b, w_down):
    S = xn.shape[1]
    u = xn @ w_up
    up = jnp.pad(u, ((0, 0), (CONV_WIDTH - 1, 0), (0, 0)))
    c = conv_b + conv_w[0] * up[:, 0:S]
    for tap in range(1, CONV_WIDTH):
        c = c + conv_w[tap] * up[:, tap:tap + S]
    gate, val = c[..., :D_FF], c[..., D_FF:]
    return (jax.nn.silu(gate) * val) @ w_down


def setup_inputs(seed: int = 0) -> dict:
    key = jax.random.key(seed)
    ks = jax.random.split(key, 24)
    n_a = len(range(0, DEPTH, N_MIXERS))
    n_b = DEPTH - n_a
    nrm = lambda k, shape, s: jax.random.normal(k, shape, jnp.float32) * s
    H, G, hd = NSA_HEADS, NSA_KV_GROUPS, NSA_HEAD_DIM
    Hm = MLSTM_HEADS
    offset = jax.random.randint(ks[1], (BATCH,), 0, 1024, dtype=jnp.int32)
    positions = offset[:, None] + jnp.arange(SEQ, dtype=jnp.int32)[None, :]
    f_bias = jnp.linspace(3.0, 6.0, Hm, dtype=jnp.float32)[None, :] + nrm(ks[11], (n_b, Hm), 0.1)
    i_bias = nrm(ks[12], (n_b, Hm), 0.1)
    return {
        "x": nrm(ks[0], (BATCH, SEQ, D_MODEL), 1.0),
        "positions": positions,
        "nsa_w_in": nrm(ks[2], (n_a, D_MODEL, NSA_IN_DIM), D_MODEL ** -0.5),
        "nsa_gate_b": nrm(ks[3], (n_a, 3, H), 0.1),
        "nsa_cmp_pe": nrm(ks[4], (n_a, 2, CMP_BLOCK, hd), 0.5),
        "nsa_cmp_w1": nrm(ks[5], (n_a, 2, CMP_BLOCK, hd, hd), (CMP_BLOCK * hd) ** -0.5),
        "nsa_cmp_w2": nrm(ks[6], (n_a, 2, hd, hd), hd ** -0.5),
        "nsa_w_out": nrm(ks[7], (n_a, H * hd, D_MODEL), (H * hd) ** -0.5),
        "mlstm_w_in": nrm(ks[8], (n_b, D_MODEL, MLSTM_IN_DIM), D_MODEL ** -0.5),
        "mlstm_gate_b": jnp.stack([i_bias, f_bias], axis=1),
        "mlstm_head_norm": 1.0 + nrm(ks[9], (n_b, Hm, MLSTM_V_DIM), 0.02),
        "mlstm_w_out": nrm(ks[10], (n_b, Hm * MLSTM_V_DIM, D_MODEL), (Hm * MLSTM_V_DIM) ** -0.5),
        "norm_mix": 1.0 + nrm(ks[13], (DEPTH, D_MODEL), 0.02),
        "norm_ffn": 1.0 + nrm(ks[14], (DEPTH, D_MODEL), 0.02),
        "ffn_w_up": nrm(ks[15], (DEPTH, D_MODEL, 2 * D_FF), D_MODEL ** -0.5),
        "ffn_conv_w": nrm(ks[16], (DEPTH, CONV_WIDTH, 2 * D_FF), CONV_WIDTH ** -0.5),
        "ffn_conv_b": nrm(ks[17], (DEPTH, 2 * D_FF), 0.02),
        "ffn_w_down": nrm(ks[18], (DEPTH, D_FF, D_MODEL), D_FF ** -0.5),
        "norm_final": 1.0 + nrm(ks[19], (D_MODEL,), 0.02),
    }


def reference(x, positions, nsa_w_in, nsa_gate_b, nsa_cmp_pe, nsa_cmp_w1, nsa_cmp_w2, nsa_w_out,
              mlstm_w_in, mlstm_gate_b, mlstm_head_norm, mlstm_w_out,
              norm_mix, norm_ffn, ffn_w_up, ffn_conv_w, ffn_conv_b, ffn_w_down, norm_final):
    for i in range(DEPTH):
        xn = rms_norm(x, norm_mix[i])
        j = i // N_MIXERS
        if i % N_MIXERS == 0:
            y = nsa_mixer(xn, positions, nsa_w_in[j], nsa_gate_b[j], nsa_cmp_pe[j],
                          nsa_cmp_w1[j], nsa_cmp_w2[j], nsa_w_out[j])
        else:
            y = mlstm_mixer(xn, mlstm_w_in[j], mlstm_gate_b[j], mlstm_head_norm[j], mlstm_w_out[j])
        x = x + y
        x = x + conv_glu_ffn(rms_norm(x, norm_ffn[i]), ffn_w_up[i], ffn_conv_w[i],
                             ffn_conv_b[i], ffn_w_down[i])
    return rms_norm(x, norm_final)
```

```python
import numpy as np
import concourse.bass as bass
import concourse.mybir as mybir
from concourse.bass_utils import run_bass_kernel_spmd

F32 = mybir.dt.float32
BF16 = mybir.dt.bfloat16
I32 = mybir.dt.int32
AF = mybir.ActivationFunctionType
ALU = mybir.AluOpType
AX = mybir.AxisListType


class T:
    def __init__(self, kb, name, handle):
        self.kb = kb
        self.name = name
        self.h = handle
        self.w = {}
        self.r = {}
        self.dsem = None
        self.dcnt = 0

    def __getitem__(self, idx):
        return V(self, self.h[idx])

    def ap(self):
        return V(self, self.h[:])


class V:
    def __init__(self, t, ap):
        self.t = t
        self.ap = ap

    def __getitem__(self, idx):
        return V(self.t, self.ap[idx])

    def re(self, pat, **kw):
        return V(self.t, self.ap.rearrange(pat, **kw))

    def bc(self, shape):
        return V(self.t, self.ap.broadcast_to(shape))


def _ap(x):
    return x.ap if isinstance(x, V) else x


class Eng:
    def __init__(self, kb, name, eng, lazy=False):
        self.kb = kb
        self.name = name
        self.e = eng
        self.sem = kb.nc.alloc_semaphore("s_" + name)
        self.cnt = 0
        self.seen = {}
        self.lazy = lazy
        self.instrs = []
        self.inc_idx = []


class KB:
    def __init__(self):
        nc = bass.Bass("TRN2", target_bir_lowering=False)
        self.nc = nc
        self.E = {
            "pe": Eng(self, "pe", nc.tensor, lazy=True),
            "act": Eng(self, "act", nc.scalar),
            "dve": Eng(self, "dve", nc.vector),
            "pool": Eng(self, "pool", nc.gpsimd),
            "sp": Eng(self, "sp", nc.sync),
        }
        self.sems = {}
        for e in self.E.values():
            self.sems[e.name] = e.sem
        self.n_t = 0
        self.dtot = {}
        self.ninstr = 0

    def sb(self, name, shape, dt):
        return T(self, name, self.nc.alloc_sbuf_tensor("sb_" + name, list(shape), dt))

    def ps(self, name, shape, dt=F32):
        return T(self, name, self.nc.alloc_psum_tensor("ps_" + name, list(shape), dt))

    def dram(self, name, shape, dt, kind="Internal"):
        h = self.nc.dram_tensor(name, list(shape), dt, kind=kind)
        return T(self, name, h.ap())

    def _resolve(self, key, val):
        if key == "pe":
            pe = self.E["pe"]
            idx = val
            import bisect
            j = bisect.bisect_left(pe.inc_idx, idx)
            if j < len(pe.inc_idx):
                return "pe", pe.instrs[pe.inc_idx[j]][1]
            ins = pe.instrs[idx]
            pe.cnt += 1
            ins[0].then_inc(pe.sem, 1)
            ins[1] = pe.cnt
            pe.inc_idx.append(idx)
            return "pe", pe.cnt
        return key, val

    def _waits(self, eng, reads, writes):
        need = {}
        for t in reads:
            for k, v in t.w.items():
                need[k] = max(need.get(k, -1), v)
        for t in writes:
            for k, v in t.w.items():
                need[k] = max(need.get(k, -1), v)
            for k, v in t.r.items():
                need[k] = max(need.get(k, -1), v)
        for k, v in need.items():
            if k == "pe" and eng.name == "pe":
                continue
            sname, val = self._resolve(k, v)
            if sname.startswith("d_"):
                val = max(val, self.dtot[sname])
            if eng.seen.get(sname, 0) >= val:
                continue
            eng.e.wait_ge(self.sems[sname], val)
            eng.seen[sname] = val

    def _record(self, key, val, reads, writes):
        for t in reads:
            t.r[key] = max(t.r.get(key, -1), val)
        for t in writes:
            t.w[key] = max(t.w.get(key, -1), val)

    @staticmethod
    def _ts(vs):
        out = []
        for v in vs:
            if isinstance(v, V) and v.t is not None and v.t not in out:
                out.append(v.t)
        return out

    def op(self, engname, fn, reads, writes):
        eng = self.E[engname]
        rt, wt = self._ts(reads), self._ts(writes)
        self._waits(eng, rt, wt)
        ins = fn(eng.e)
        self.ninstr += 1
        if eng.lazy:
            eng.instrs.append([ins, None])
            self._record(eng.name, len(eng.instrs) - 1, rt, wt)
        else:
            eng.cnt += 1
            ins.then_inc(eng.sem, 1)
            self._record(eng.name, eng.cnt, rt, wt)
        return ins

    def dma(self, q, out, in_, **kw):
        eng = self.E[q]
        rt, wt = self._ts([in_]), self._ts([out])
        self._waits(eng, rt, wt)
        owner = wt[0] if wt else rt[0]
        if owner.dsem is None:
            nm = "d_%d" % len(self.sems)
            owner.dsem = nm
            self.sems[nm] = self.nc.alloc_semaphore(nm)
            self.dtot[nm] = 0
        ins = eng.e.dma_start(out=_ap(out), in_=_ap(in_), **kw)
        ins.then_inc(self.sems[owner.dsem], 16)
        self.dtot[owner.dsem] += 16
        self.ninstr += 1
        self._record(owner.dsem, self.dtot[owner.dsem], rt, wt)

    def mm(self, out, lhsT, rhs, start=True, stop=True, **kw):
        return self.op("pe", lambda e: e.matmul(_ap(out), _ap(lhsT), _ap(rhs), start=start, stop=stop, **kw),
                       [lhsT, rhs], [out])

    def transpose(self, out, in_, ident):
        return self.op("pe", lambda e: e.transpose(_ap(out), _ap(in_), _ap(ident)), [in_, ident], [out])

    def act(self, out, in_, func, bias=None, scale=1.0, eng="act", accum_out=None):
        kw = {}
        rd = [in_]
        if bias is not None:
            kw["bias"] = _ap(bias)
            rd.append(bias)
        if isinstance(scale, V):
            rd.append(scale)
        kw["scale"] = _ap(scale)
        wr = [out]
        if accum_out is not None:
            kw["accum_out"] = _ap(accum_out)
            wr.append(accum_out)
        return self.op(eng, lambda e: e.activation(out=_ap(out), in_=_ap(in_), func=func, **kw), rd, wr)

    def tt(self, out, a, b, op, eng="dve"):
        return self.op(eng, lambda e: e.tensor_tensor(out=_ap(out), in0=_ap(a), in1=_ap(b), op=op), [a, b], [out])

    def ts(self, out, a, s1, op0, s2=None, op1=None, eng="dve", accum_out=None):
        rd = [a] + [s for s in (s1, s2) if isinstance(s, V)]
        kw = {}
        if op1 is not None:
            kw["op1"] = op1
        wr = [out]
        if accum_out is not None:
            kw["accum_out"] = _ap(accum_out)
            wr.append(accum_out)
        return self.op(eng, lambda e: e.tensor_scalar(out=_ap(out), in0=_ap(a), scalar1=_ap(s1), scalar2=_ap(s2),
                                                      op0=op0, **kw), rd, wr)

    def stt(self, out, a, s, b, op0, op1, eng="dve"):
        rd = [a, b] + ([s] if isinstance(s, V) else [])
        return self.op(eng, lambda e: e.scalar_tensor_tensor(out=_ap(out), in0=_ap(a), scalar=_ap(s), in1=_ap(b),
                                                             op0=op0, op1=op1), rd, [out])

    def copy(self, out, in_, eng="dve"):
        if eng == "act":
            return self.op(eng, lambda e: e.copy(out=_ap(out), in_=_ap(in_)), [in_], [out])
        return self.op(eng, lambda e: e.tensor_copy(out=_ap(out), in_=_ap(in_)), [in_], [out])

    def memset(self, out, val, eng="dve"):
        return self.op(eng, lambda e: e.memset(_ap(out), val), [], [out])

    def recip(self, out, in_):
        return self.op("dve", lambda e: e.reciprocal(out=_ap(out), in_=_ap(in_)), [in_], [out])

    def max8(self, out, in_):
        return self.op("dve", lambda e: e.max(out=_ap(out), in_=_ap(in_)), [in_], [out])

    def finish(self, outs):
        eng = self.E["sp"]
        self._waits(eng, self._ts(outs), [])

import math

D = 2048; DFF = 5632; NKT = 16; NJ = 44; EPS = 1e-6

def emit_rmsnorm(kb, xs, g, xn, ones, ps_ss, rstd, sqs, ncols):
    chunks = [(c, min(c + 512, ncols)) for c in range(0, ncols, 512)]
    for kt in range(NKT):
        sq = sqs[kt % len(sqs)]
        kb.act(sq[:, 0:ncols], xs[kt], AF.Square)
        for (a, b) in chunks:
            kb.mm(ps_ss[:, a:b], ones.ap(), sq[:, a:b], start=(kt == 0), stop=(kt == NKT - 1))
    kb.ts(rstd[:, 0:ncols], ps_ss[:, 0:ncols], 1.0 / D, ALU.mult, EPS, ALU.add)
    kb.act(rstd[:, 0:ncols], rstd[:, 0:ncols], AF.Sqrt)
    kb.recip(rstd[:, 0:ncols], rstd[:, 0:ncols])
    for kt in range(NKT):
        kb.stt(xn[:, kt, 0:ncols], xs[kt], g[:, kt:kt + 1], rstd[:, 0:ncols], ALU.mult, ALU.mult)


def build_ffn(final=False):
    kb = KB(); nc = kb.nc
    hT = nc.dram_tensor("hT", [D, 1026], F32, kind="ExternalInput").ap()
    y0T = nc.dram_tensor("y0T", [D, 1026], F32, kind="ExternalInput").ap()
    y1T = nc.dram_tensor("y1T", [D, 1026], F32, kind="ExternalInput").ap()
    g_d = nc.dram_tensor("g", [128, NKT], F32, kind="ExternalInput").ap()
    gf_d = nc.dram_tensor("gf", [128, NKT], F32, kind="ExternalInput").ap()
    cw_d = nc.dram_tensor("cw", [128, 3, 2 * NJ], F32, kind="ExternalInput").ap()
    cb_d = nc.dram_tensor("cb", [128, 2 * NJ], F32, kind="ExternalInput").ap()
    wup = nc.dram_tensor("wup", [D, 2 * DFF], F32, kind="ExternalInput").ap()
    wdn = nc.dram_tensor("wdn", [DFF, D], F32, kind="ExternalInput").ap()
    out = kb.dram("out", [D, 1024], F32, kind="ExternalOutput")

    g = kb.sb("g", [128, NKT], F32); gf = kb.sb("gf", [128, NKT], F32)
    cw = kb.sb("cw", [128, 3, 2 * NJ], F32); cb = kb.sb("cb", [128, 2 * NJ], F32)
    ones = kb.sb("ones", [128, 128], F32)
    kb.dma("sp", g.ap(), g_d); kb.dma("sp", gf.ap(), gf_d)
    kb.dma("sp", cw.ap(), cw_d); kb.dma("sp", cb.ap(), cb_d)
    kb.memset(ones.ap(), 1.0)

    hbuf = [kb.sb(f"hb{k}", [128, 514], F32) for k in range(NKT)]
    xn = kb.sb("xn", [128, NKT, 514], BF16)
    hg = kb.sb("hg", [128, NJ, 512], BF16)
    ring = [kb.sb(f"ring{i}", [128, 8192], BF16) for i in range(4)]
    sqs = [kb.sb(f"sq{i}", [128, 514], F32) for i in range(2)]
    rstd = kb.sb("rstd", [128, 514], F32)
    cg = [kb.sb(f"cg{i}", [128, 512], F32) for i in range(2)]
    cv = [kb.sb(f"cv{i}", [128, 512], F32) for i in range(2)]
    sg = [kb.sb(f"sg{i}", [128, 512], F32) for i in range(2)]
    ps = [kb.ps(f"ps{i}", [128, 1024], F32) for i in range(4)]
    ri = 0
    hTv = hT.rearrange("(kt p) t -> kt p t", p=128)
    y0v = y0T.rearrange("(kt p) t -> kt p t", p=128)
    y1v = y1T.rearrange("(kt p) t -> kt p t", p=128)
    ya = [kb.sb(f"ya{i}", [128, 514], F32) for i in range(2)]
    yb = [kb.sb(f"yb{i}", [128, 514], F32) for i in range(2)]
    wupv = wup.rearrange("(kt p) f -> p kt f", p=128)
    wdnv = wdn.rearrange("(j p) d -> p j d", p=128)
    outv = out.ap().re("(kt p) t -> kt p t", p=128)
    for hf in range(2):
        t0 = 512 * hf
        for kt in range(NKT):
            kb.dma("sp", hbuf[kt].ap(), hTv[kt, :, t0:t0 + 514])
            kb.dma("sp", ya[kt % 2].ap(), y0v[kt, :, t0:t0 + 514])
            kb.dma("sp", yb[kt % 2].ap(), y1v[kt, :, t0:t0 + 514])
            kb.tt(ya[kt % 2].ap(), ya[kt % 2].ap(), yb[kt % 2].ap(), ALU.add, eng="pool")
            kb.tt(hbuf[kt].ap(), hbuf[kt].ap(), ya[kt % 2].ap(), ALU.add, eng="pool")
        emit_rmsnorm(kb, [hbuf[kt].ap() for kt in range(NKT)], g.ap(), xn, ones, ps[0], rstd, sqs, 514)
        for st in range(NJ // 2):
            rb = ring[ri % 4]; ri += 1
            wg = rb[:, 0:4096].re("p (kt f) -> p kt f", f=256)
            wv = rb[:, 4096:8192].re("p (kt f) -> p kt f", f=256)
            kb.dma("pool", wg, wupv[:, :, 256 * st:256 * st + 256])
            kb.dma("pool", wv, wupv[:, :, DFF + 256 * st:DFF + 256 * st + 256])
            for jj in range(2):
                j = 2 * st + jj
                pg = ps[(2 * j) % 4]; pv = ps[(2 * j + 1) % 4]
                for (pp, ww) in ((pg, wg), (pv, wv)):
                    for kt in range(NKT):
                        kb.mm(pp[:, 0:512], ww[:, kt, 128 * jj:128 * jj + 128], xn[:, kt, 0:512],
                              start=(kt == 0), stop=(kt == NKT - 1))
                    for kt in range(NKT):
                        kb.mm(pp[:, 512:514], ww[:, kt, 128 * jj:128 * jj + 128], xn[:, kt, 512:514],
                              start=(kt == 0), stop=(kt == NKT - 1))
                b = j % 2
                for (pp, cc, fo) in ((pg, cg[b], j), (pv, cv[b], NJ + j)):
                    kb.act(cc.ap(), pp[:, 2:514], AF.Identity, bias=cb[:, fo:fo + 1], scale=cw[:, 2, fo:fo + 1])
                    kb.stt(cc.ap(), pp[:, 1:513], cw[:, 1, fo:fo + 1], cc.ap(), ALU.mult, ALU.add)
                    kb.stt(cc.ap(), pp[:, 0:512], cw[:, 0, fo:fo + 1], cc.ap(), ALU.mult, ALU.add)
                kb.act(sg[b].ap(), cg[b].ap(), AF.Silu)
                kb.tt(hg[:, j, :], sg[b].ap(), cv[b].ap(), ALU.mult)
        for dt in range(NKT):
            rb = ring[ri % 4]; ri += 1
            wd = rb[:, 0:NJ * 128].re("p (j d) -> p j d", d=128)
            kb.dma("pool", wd, wdnv[:, :, 128 * dt:128 * dt + 128])
            pp = ps[dt % 4]
            for j in range(NJ):
                kb.mm(pp[:, 0:512], wd[:, j, :], hg[:, j, :], start=(j == 0), stop=(j == NJ - 1))
            kb.tt(hbuf[dt][:, 2:514], hbuf[dt][:, 2:514], pp[:, 0:512], ALU.add)
            if not final:
                kb.dma("sp", outv[dt][:, t0:t0 + 512], hbuf[dt][:, 2:514])
        if final:
            for kt in range(NKT):
                sq = sqs[kt % 2]
                kb.act(sq[:, 0:512], hbuf[kt][:, 2:514], AF.Square)
                kb.mm(ps[0][:, 0:512], ones.ap(), sq[:, 0:512], start=(kt == 0), stop=(kt == NKT - 1))
            kb.ts(rstd[:, 0:512], ps[0][:, 0:512], 1.0 / D, ALU.mult, EPS, ALU.add)
            kb.act(rstd[:, 0:512], rstd[:, 0:512], AF.Sqrt)
            kb.recip(rstd[:, 0:512], rstd[:, 0:512])
            for kt in range(NKT):
                kb.stt(hbuf[kt][:, 2:514], hbuf[kt][:, 2:514], gf[:, kt:kt + 1], rstd[:, 0:512], ALU.mult, ALU.mult)
                kb.dma("sp", outv[kt][:, t0:t0 + 512], hbuf[kt][:, 2:514])
    kb.finish([out.ap()])
    return kb


def ffn_inputs(hT_halo, y0_halo, y1_halo, g, gf, conv_w, conv_b, w_up, w_down):
    return {
        "hT": np.ascontiguousarray(hT_halo),
        "y0T": np.ascontiguousarray(y0_halo),
        "y1T": np.ascontiguousarray(y1_halo),
        "g": np.ascontiguousarray(g.reshape(NKT, 128).T),
        "gf": np.ascontiguousarray(gf.reshape(NKT, 128).T),
        "cw": np.ascontiguousarray(conv_w.reshape(3, 2 * NJ, 128).transpose(2, 0, 1)),
        "cb": np.ascontiguousarray(conv_b.reshape(2 * NJ, 128).T),
        "wup": w_up, "wdn": w_down,
    }


S = 2048
NH = 4
DK = 128; DV = 256
CAP = 15.0


def emit_norm_half(kb, xTv, t0, nt, g, xn, ones, ps_ss, rstd, xb, sqs):
    chunks = [(c, min(c + 512, nt)) for c in range(0, nt, 512)]
    for kt in range(NKT):
        b = xb[kt % len(xb)]
        kb.dma("sp", b[:, 0:nt], xTv[kt, :, t0:t0 + nt])
        sq = sqs[kt % len(sqs)]
        kb.act(sq[:, 0:nt], b[:, 0:nt], AF.Square)
        for (a, e) in chunks:
            kb.mm(ps_ss[:, a:e], ones.ap(), sq[:, a:e], start=(kt == 0), stop=(kt == NKT - 1))
    kb.ts(rstd[:, 0:nt], ps_ss[:, 0:nt], 1.0 / D, ALU.mult, EPS, ALU.add)
    kb.act(rstd[:, 0:nt], rstd[:, 0:nt], AF.Sqrt)
    kb.recip(rstd[:, 0:nt], rstd[:, 0:nt])
    for kt in range(NKT):
        b = xb[kt % len(xb)]
        kb.dma("sp", b[:, 0:nt], xTv[kt, :, t0:t0 + nt])
        kb.stt(xn[:, kt, 0:nt], b[:, 0:nt], g[:, kt:kt + 1], rstd[:, 0:nt], ALU.mult, ALU.mult)


def build_mlstm():
    kb = KB(); nc = kb.nc
    xT = nc.dram_tensor("xT", [D, S], F32, kind="ExternalInput").ap()
    g_d = nc.dram_tensor("g", [128, NKT], F32, kind="ExternalInput").ap()
    win = nc.dram_tensor("win", [D, 3080], F32, kind="ExternalInput").ap()
    gb_d = nc.dram_tensor("gb", [128, 8], F32, kind="ExternalInput").ap()
    hn_d = nc.dram_tensor("hn", [128, 8], F32, kind="ExternalInput").ap()
    wout = nc.dram_tensor("wout", [NH * DV, D], F32, kind="ExternalInput").ap()
    id_d = nc.dram_tensor("ident", [128, 128], F32, kind="ExternalInput").ap()
    tri_d = nc.dram_tensor("tri", [128, 128], F32, kind="ExternalInput").ap()
    yT = kb.dram("yT", [D, S], F32, kind="ExternalOutput")

    g = kb.sb("g", [128, NKT], F32); gb = kb.sb("gb", [128, 8], F32); hn = kb.sb("hn", [128, 8], F32)
    ident = kb.sb("ident", [128, 128], F32); tri = kb.sb("tri", [128, 128], F32)
    trib = kb.sb("trib", [128, 128], BF16)
    ones = kb.sb("ones", [128, 128], F32); onesb = kb.sb("onesb", [128, 128], BF16)
    for (t, d) in ((g, g_d), (gb, gb_d), (hn, hn_d), (ident, id_d), (tri, tri_d)):
        kb.dma("sp", t.ap(), d)
    kb.memset(ones.ap(), 1.0); kb.memset(onesb.ap(), 1.0)
    kb.copy(trib.ap(), tri.ap())

    xn = kb.sb("xn", [128, NKT, 1024], BF16)
    xb = [kb.sb(f"xb{i}", [128, 1024], F32) for i in range(3)]
    sqs = [kb.sb(f"sq{i}", [128, 1024], F32) for i in range(2)]
    rstd = kb.sb("rstd", [128, 1024], F32)
    qT = kb.sb("qT", [128, NH, S], BF16)
    kT = kb.sb("kT", [128, NH, S], BF16)
    vt = kb.sb("vt", [128, 16, NH * DV], BF16)
    og = kb.sb("og", [128, 2 * NH, S], BF16)
    gates = kb.sb("gates", [128, 16, 8], F32)
    ring = [kb.sb(f"ring{i}", [128, 4096], BF16) for i in range(3)]
    ps = [kb.ps(f"ps{i}", [128, 512], F32) for i in range(8)]
    ri = 0; pi = 0
    xTv = xT.rearrange("(kt p) t -> kt p t", p=128)
    winv = win.rearrange("(kt p) f -> p kt f", p=128)

    for half in range(2):
        t0 = 1024 * half
        class _PS2:
            pass
        for kt in range(NKT):
            b = xb[kt % 3]
            kb.dma("sp", b.ap(), xTv[kt, :, t0:t0 + 1024])
            sq = sqs[kt % 2]
            kb.act(sq.ap(), b.ap(), AF.Square)
            for c in range(2):
                kb.mm(ps[c].ap(), ones.ap(), sq[:, 512 * c:512 * c + 512], start=(kt == 0), stop=(kt == NKT - 1))
        for c in range(2):
            kb.ts(rstd[:, 512 * c:512 * c + 512], ps[c].ap(), 1.0 / D, ALU.mult, EPS, ALU.add)
        kb.act(rstd.ap(), rstd.ap(), AF.Sqrt)
        kb.recip(rstd.ap(), rstd.ap())
        for kt in range(NKT):
            b = xb[kt % 3]
            kb.dma("sp", b.ap(), xTv[kt, :, t0:t0 + 1024])
            kb.stt(xn[:, kt, :], b.ap(), g[:, kt:kt + 1], rstd.ap(), ALU.mult, ALU.mult)
        for st in range(8):
            rb = ring[ri % 3]; ri += 1
            c0 = 256 * st if st < 4 else 2048 + 256 * (st - 4)
            w = rb.ap().re("p (kt f) -> p kt f", f=256)
            kb.dma("pool", w, winv[:, :, c0:c0 + 256])
            for jj in range(2):
                ft = 2 * st + jj
                for c in range(2):
                    pp = ps[pi % 8]; pi += 1
                    for kt in range(NKT):
                        kb.mm(pp.ap(), w[:, kt, 128 * jj:128 * jj + 128], xn[:, kt, 512 * c:512 * c + 512],
                              start=(kt == 0), stop=(kt == NKT - 1))
                    tt0 = t0 + 512 * c
                    if ft < 4:
                        kb.act(qT[:, ft, tt0:tt0 + 512], pp.ap(), AF.Copy, scale=DK ** -0.5)
                    elif ft < 8:
                        kb.copy(kT[:, ft - 4, tt0:tt0 + 512], pp.ap())
                    else:
                        kb.act(og[:, ft - 8, tt0:tt0 + 512], pp.ap(), AF.Sigmoid)
        for st in range(4):
            rb = ring[ri % 3]; ri += 1
            w = rb.ap().re("p (kt f) -> p kt f", f=256)
            kb.dma("pool", w, winv[:, :, 1024 + 256 * st:1024 + 256 * st + 256])
            for tt in range(8):
                pp = ps[pi % 8]; pi += 1
                for kt in range(NKT):
                    kb.mm(pp[:, 0:256], xn[:, kt, 128 * tt:128 * tt + 128], w[:, kt, :],
                          start=(kt == 0), stop=(kt == NKT - 1))
                kb.copy(vt[:, 8 * half + tt, 256 * st:256 * st + 256], pp[:, 0:256], eng=("act" if tt % 2 else "dve"))
        rb = ring[ri % 3]; ri += 1
        wgt = rb[:, 0:NKT * 8].re("p (kt f) -> p kt f", f=8)
        kb.dma("pool", wgt, winv[:, :, 3072:3080])
        for tt in range(8):
            pp = ps[pi % 8]; pi += 1
            for kt in range(NKT):
                kb.mm(pp[:, 0:8], xn[:, kt, 128 * tt:128 * tt + 128], wgt[:, kt, :], start=(kt == 0), stop=(kt == NKT - 1))
            kb.tt(gates[:, 8 * half + tt, :], pp[:, 0:8], gb.ap(), ALU.add)

    z = kb.sb("z", [128, 16, 8], F32)
    kb.act(z.ap(), gates.ap(), AF.Tanh, scale=1.0 / CAP)
    kb.ts(z.ap(), z.ap(), CAP, ALU.mult)
    lf = kb.sb("lf", [128, 16, 4], F32)
    kb.act(lf.ap(), z[:, :, 4:8], AF.Exp, scale=-1.0)
    kb.act(lf.ap(), lf.ap(), AF.Ln, bias=1.0)
    kb.ts(lf.ap(), lf.ap(), -1.0, ALU.mult)
    fc = kb.sb("fc", [128, 16, 4], F32)
    gcol = kb.sb("gcol", [128, 16, 4], F32)
    for i in range(16):
        pp = ps[pi % 8]; pi += 1
        for j in range(i + 1):
            kb.mm(pp[:, 0:4], (tri if j == i else ones).ap(), lf[:, j, :], start=(j == 0), stop=(j == i))
        kb.copy(fc[:, i, :], pp[:, 0:4])
    kb.tt(gcol.ap(), z[:, :, 0:4], fc.ap(), ALU.subtract)

    fb = [kb.sb("fb0", [128, S], F32)] * 2
    dts = [kb.sb(f"dt{i}", [128, 512], F32) for i in range(3)]
    pts = [kb.sb(f"pt{i}", [128, 512], BF16) for i in range(3)]
    hbuf = [xb[2].ap().re("p (a t) -> p a t", a=2), rstd.ap().re("p (a t) -> p a t", a=2)]
    rden = [xb[0][:, 0:512], xb[1][:, 0:512]]
    hsq = [sqs[0][:, 0:512], sqs[1][:, 0:512]]
    fcb = kb.sb("fcb", [128, 128], F32)
    ps_s = [ps[0], ps[1]]; sets = [(ps[2], ps[3], ps[4]), (ps[5], ps[6], ps[7])]
    si = 0; di = 0; qi = 0
    for h in range(NH):
        F = fb[h % 2]
        for i in range(16):
            pp = ps_s[si % 2]; si += 1
            kb.copy(fcb.ap(), fc[:, i, h:h + 1].bc([128, 128]), eng="pool")
            kb.mm(pp[:, 0:128], fcb.ap(), ident.ap(), start=True, stop=True)
            kb.copy(F[:, 128 * i:128 * i + 128], pp[:, 0:128])
        for qc in range(4):
            n0, n1, dn = sets[qi % 2]; hb = hbuf[qi % 2]; rd = rden[qi % 2]; qi += 1
            nk = 4 * qc + 4
            for kt in range(nk):
                r = kt - 4 * qc
                c0 = 128 * r if r > 0 else 0
                sp_ = ps_s[si % 2]; si += 1
                dtt = dts[di % 3]; pt = pts[di % 3]; di += 1
                kb.mm(sp_[:, c0:512], kT[:, h, 128 * kt:128 * kt + 128], qT[:, h, 512 * qc + c0:512 * qc + 512],
                      start=True, stop=True)
                kb.act(dtt[:, c0:512], F[:, 512 * qc + c0:512 * qc + 512], AF.Exp, bias=gcol[:, kt, h:h + 1])
                if r >= 0:
                    kb.tt(dtt[:, c0:c0 + 128], dtt[:, c0:c0 + 128], tri.ap(), ALU.mult, eng="pool")
                kb.tt(pt[:, c0:512], sp_[:, c0:512], dtt[:, c0:512], ALU.mult)
                for (acc, lh) in ((n0, vt[:, kt, DV * h:DV * h + 128]), (n1, vt[:, kt, DV * h + 128:DV * h + 256]),
                                  (dn, onesb.ap())):
                    kb.mm(acc[:, c0:512], lh, pt[:, c0:512], start=(kt == 0), stop=(kt == nk - 1))
            kb.act(rd, dn.ap(), AF.Abs)
            kb.ts(rd, rd, 1.0, ALU.max)
            kb.recip(rd, rd)
            kb.tt(hb[:, 0, :], n0.ap(), rd, ALU.mult)
            kb.tt(hb[:, 1, :], n1.ap(), rd, ALU.mult)
            for dvt in range(2):
                sq = hsq[dvt]
                kb.act(sq, hb[:, dvt, :], AF.Square)
                kb.mm(dn.ap(), ones.ap(), sq, start=(dvt == 0), stop=(dvt == 1))
            kb.ts(rd, dn.ap(), 1.0 / DV, ALU.mult, EPS, ALU.add)
            kb.act(rd, rd, AF.Sqrt)
            kb.recip(rd, rd)
            for dvt in range(2):
                kb.stt(hb[:, dvt, :], hb[:, dvt, :], hn[:, 2 * h + dvt:2 * h + dvt + 1], rd, ALU.mult, ALU.mult)
                o = og[:, 2 * h + dvt, 512 * qc:512 * qc + 512]
                kb.tt(o, hb[:, dvt, :], o, ALU.mult)
    woutv = wout.rearrange("(c p) d -> p c d", p=128)
    yTv = yT.ap().re("(kt p) t -> kt p t", p=128)
    ob = dts
    oi = 0
    for st in range(8):
        rb = ring[ri % 3]; ri += 1
        w = rb[:, 0:2048].re("p (c f) -> p c f", f=256)
        kb.dma("pool", w, woutv[:, :, 256 * st:256 * st + 256])
        for jj in range(2):
            dt_ = 2 * st + jj
            for qc in range(4):
                pp = ps[pi % 8]; pi += 1
                for c in range(8):
                    kb.mm(pp.ap(), w[:, c, 128 * jj:128 * jj + 128], og[:, c, 512 * qc:512 * qc + 512],
                          start=(c == 0), stop=(c == 7))
                o = ob[oi % 3]; oi += 1
                kb.copy(o.ap(), pp.ap(), eng=("act" if oi % 2 else "dve"))
                kb.dma("sp", yTv[dt_][:, 512 * qc:512 * qc + 512], o.ap())
    kb.finish([yT.ap()])
    return kb


def mlstm_inputs(xT_b, g, w_in, gate_b, head_norm, w_out, p):
    hs = slice(4 * p, 4 * p + 4)
    o1 = 8 * 128; o2 = 2 * o1; o3 = o2 + 8 * 256; o4 = o3 + 8 * 256; o5 = o4 + 8
    cols = np.concatenate([
        np.arange(512 * p, 512 * p + 512), o1 + np.arange(512 * p, 512 * p + 512),
        o2 + np.arange(1024 * p, 1024 * p + 1024), o3 + np.arange(1024 * p, 1024 * p + 1024),
        o4 + np.arange(4 * p, 4 * p + 4), o5 + np.arange(4 * p, 4 * p + 4)])
    gbv = np.concatenate([gate_b[0, hs], gate_b[1, hs]])[None, :].repeat(128, 0)
    hnv = head_norm[hs].reshape(4, 2, 128).transpose(2, 0, 1).reshape(128, 8)
    return {
        "xT": np.ascontiguousarray(xT_b),
        "g": np.ascontiguousarray(g.reshape(NKT, 128).T),
        "win": np.ascontiguousarray(w_in[:, cols]),
        "gb": np.ascontiguousarray(gbv.astype(np.float32)),
        "hn": np.ascontiguousarray(hnv.astype(np.float32)),
        "wout": np.ascontiguousarray(w_out[1024 * p:1024 * p + 1024]),
        "ident": np.eye(128, dtype=np.float32),
        "tri": np.triu(np.ones((128, 128), np.float32)),
    }


import math

HD = 128; NHC = 8
NEG = -30000.0
TWO_PI = 2.0 * math.pi


def build_nsa(phase=99):
    kb = KB(); nc = kb.nc
    def din(name, shape, dt=F32):
        return nc.dram_tensor(name, list(shape), dt, kind="ExternalInput").ap()
    xT = din("xT", [D, S]); g_d = din("g", [128, NKT])
    win = din("win", [D, 1816]); gb_d = din("gb", [24, 1])
    pos_d = din("pos", [32, S], I32)
    pe_d = din("pe", [2, 32, 128]); w1_d = din("w1", [2, 32, 128, 128]); w2_d = din("w2", [2, 128, 128])
    wout = din("wout", [NHC * HD, D])
    id_d = din("ident", [128, 128]); cmpneg_d = din("cmpneg", [128, S]); cneg_d = din("cneg", [128, 128])
    aneg_d = din("aneg", [128, 128]); E_d = din("E", [128, S]); ov_d = din("ov", [128, 32])
    am_d = din("addmask", [128, 16, 32]); sel24_d = din("sel24", [128, 24 * 128]); pm_d = din("pm", [128, 32])
    rc_d = din("ropec", [32, 3])
    yT = kb.dram("yT", [D, S], F32, kind="ExternalOutput")

    g = kb.sb("g", [128, NKT], F32); gb = kb.sb("gb", [24, 1], F32)
    ident = kb.sb("ident", [128, 128], F32); identb = kb.sb("identb", [128, 128], BF16)
    cmpneg = kb.sb("cmpneg", [128, S], BF16); cneg = kb.sb("cneg", [128, 128], BF16); aneg = kb.sb("aneg", [128, 128], BF16)
    Eb = kb.sb("Eb", [128, S], BF16); ovb = kb.sb("ovb", [128, 32], BF16)
    addmask = kb.sb("addmask", [128, 16, 32], F32); sel24 = kb.sb("sel24", [128, 24 * 128], BF16)
    pmb = kb.sb("pmb", [128, 32], BF16); rc = kb.sb("rc", [32, 3], F32)
    posi = kb.sb("posi", [32, 1024], I32)
    ones = kb.sb("ones", [128, 128], F32); onesb = kb.sb("onesb", [128, 128], BF16)
    for (t, d) in ((g, g_d), (gb, gb_d), (ident, id_d), (addmask, am_d), (rc, rc_d)):
        kb.dma("sp", t.ap(), d)
    for (t, d) in ((identb, id_d), (cmpneg, cmpneg_d), (cneg, cneg_d), (aneg, aneg_d), (Eb, E_d), (ovb, ov_d),
                   (sel24, sel24_d), (pmb, pm_d)):
        kb.dma("pool", t.ap(), d)
    kb.memset(ones.ap(), 1.0); kb.memset(onesb.ap(), 1.0)

    xn = kb.sb("xn", [128, NKT * 1024], BF16)
    xnv = xn.ap().re("p (kt t) -> p kt t", t=1024)
    oT = xn.ap().re("p (h t) -> p h t", t=S)
    xb = [kb.sb(f"xb{i}", [128, 1024], F32) for i in range(3)]
    sqs = [kb.sb(f"sq{i}", [128, 1024], F32) for i in range(2)]
    rstd = kb.sb("rstd", [128, 1024], F32)
    qT = [kb.sb(f"qT{h}", [128, S], BF16) for h in range(NHC)]
    kcT = kb.sb("kcT", [128, S], BF16); vcT = kb.sb("vcT", [128, S], BF16)
    ksT = kb.sb("ksT", [128, S], BF16); kwT = kb.sb("kwT", [128, S], BF16)
    vs = kb.sb("vs", [128, 16, 128], BF16); vw = kb.sb("vw", [128, 16, 128], BF16)
    gT = kb.sb("gT", [128, S], BF16)
    kb.memset(gT.ap(), 0.0)
    cosT = kb.sb("cosT", [32, S], F32); sinT = kb.sb("sinT", [32, S], F32)
    ring = [kb.sb(f"ring{i}", [128, 4096], BF16) for i in range(3)]
    ps = [kb.ps(f"ps{i}", [128, 512], F32) for i in range(8)]
    ri = 0; pi = 0
    xTv = xT.rearrange("(kt p) t -> kt p t", p=128)
    winv = win.rearrange("(kt p) f -> p kt f", p=128)

    posf = sqs[0]; ang = sqs[1]; kf = xb[0]; mk_ = xb[1]

    def reduce_pi(a):
        kb.ts(kf[0:32, :], a, 1.0 / TWO_PI, ALU.mult)
        kb.copy(posi.ap(), kf[0:32, :])
        kb.copy(kf[0:32, :], posi.ap())
        kb.stt(a, kf[0:32, :], -TWO_PI, a, ALU.mult, ALU.add)
        kb.ts(mk_[0:32, :], a, math.pi, ALU.is_gt)
        kb.stt(a, mk_[0:32, :], -TWO_PI, a, ALU.mult, ALU.add)
        kb.ts(mk_[0:32, :], a, -math.pi, ALU.is_lt)
        kb.stt(a, mk_[0:32, :], TWO_PI, a, ALU.mult, ALU.add)
        kb.ts(a, a, -3.141592, ALU.max, 3.141592, ALU.min)

    for c in range(2):
        sl = slice(1024 * c, 1024 * c + 1024)
        kb.dma("sp", posi.ap(), pos_d[:, sl])
        kb.copy(posf[0:32, :], posi.ap())
        kb.ts(ang[0:32, :], posf[0:32, :], rc[:, 0:1], ALU.mult)
        kb.ts(posf[0:32, :], ang[0:32, :], math.pi / 2, ALU.add)
        reduce_pi(ang[0:32, :])
        kb.act(sinT[:, sl], ang[0:32, :], AF.Sin, scale=rc[:, 1:2])
        reduce_pi(posf[0:32, :])
        kb.act(cosT[:, sl], posf[0:32, :], AF.Sin)

    ropex = [kb.sb(f"ropex{i}", [32, 512], F32) for i in range(2)]
    ropet = [kb.sb(f"ropet{i}", [32, 512], F32) for i in range(2)]
    rpi = [0]

    def rope_inplace(dstT, tsl):
        i = rpi[0] % 2; rpi[0] += 1
        x_ = ropex[i]; t_ = ropet[i]
        pp = ps[7]
        kb.mm(pp[0:32, :], pmb.ap(), dstT[:, tsl], start=True, stop=True)
        kb.tt(t_.ap(), dstT[0:32, tsl], cosT[:, tsl], ALU.mult)
        kb.tt(x_.ap(), pp[0:32, :], sinT[:, tsl], ALU.mult)
        kb.tt(dstT[0:32, tsl], t_.ap(), x_.ap(), ALU.add)

    for half in range(2):
        t0 = 1024 * half
        for kt in range(NKT):
            b = xb[kt % 3]
            kb.dma("sp", b.ap(), xTv[kt, :, t0:t0 + 1024])
            sq = sqs[kt % 2]
            kb.act(sq.ap(), b.ap(), AF.Square)
            for c in range(2):
                kb.mm(ps[c].ap(), ones.ap(), sq[:, 512 * c:512 * c + 512], start=(kt == 0), stop=(kt == NKT - 1))
        for c in range(2):
            kb.ts(rstd[:, 512 * c:512 * c + 512], ps[c].ap(), 1.0 / D, ALU.mult, EPS, ALU.add)
        kb.act(rstd.ap(), rstd.ap(), AF.Sqrt)
        kb.recip(rstd.ap(), rstd.ap())
        for kt in range(NKT):
            b = xb[kt % 3]
            kb.dma("sp", b.ap(), xTv[kt, :, t0:t0 + 1024])
            kb.stt(xnv[:, kt, :], b.ap(), g[:, kt:kt + 1], rstd.ap(), ALU.mult, ALU.mult)
        for st in range(8):
            rb = ring[ri % 3]; ri += 1
            ncol = 256 if st < 7 else 24
            w = rb[:, 0:NKT * ncol].re("p (kt f) -> p kt f", f=ncol)
            kb.dma("pool", w, winv[:, :, 256 * st:256 * st + ncol])
            if st == 7:
                for c in range(2):
                    pp = ps[pi % 6]; pi += 1
                    for kt in range(NKT):
                        kb.mm(pp[0:24, :], w[:, kt, :], xnv[:, kt, 512 * c:512 * c + 512], start=(kt == 0), stop=(kt == NKT - 1))
                    kb.act(gT[0:24, t0 + 512 * c:t0 + 512 * c + 512], pp[0:24, :], AF.Sigmoid, bias=gb[:, 0:1])
                continue
            for jj in range(2):
                if st >= 5 and jj == 1:
                    dstv = vs if st == 5 else vw
                    for tt in range(8):
                        pp = ps[pi % 6]; pi += 1
                        for kt in range(NKT):
                            kb.mm(pp[:, 0:128], xnv[:, kt, 128 * tt:128 * tt + 128], w[:, kt, 128:256],
                                  start=(kt == 0), stop=(kt == NKT - 1))
                        kb.copy(dstv[:, 8 * half + tt, :], pp[:, 0:128], eng=("act" if tt % 2 else "dve"))
                    continue
                for c in range(2):
                    pp = ps[pi % 6]; pi += 1
                    for kt in range(NKT):
                        kb.mm(pp.ap(), w[:, kt, 128 * jj:128 * jj + 128], xnv[:, kt, 512 * c:512 * c + 512],
                              start=(kt == 0), stop=(kt == NKT - 1))
                    tsl = slice(t0 + 512 * c, t0 + 512 * c + 512)
                    if st < 4:
                        kb.act(qT[2 * st + jj][:, tsl], pp.ap(), AF.Copy, scale=HD ** -0.5)
                    elif st == 4:
                        kb.copy((kcT if jj == 0 else vcT)[:, tsl], pp.ap(), eng=("act" if jj else "dve"))
                    else:
                        dst = ksT if st == 5 else kwT
                        kb.copy(dst[:, tsl], pp.ap(), eng="act")
                        rope_inplace(dst, tsl)

    peb = kb.sb("peb", [128, 128], F32)
    kb.memset(peb.ap(), 0.0); peT = kb.sb("peT", [128, 32], BF16)
    cst = kb.sb("cst", [128, 1], F32)
    hu = kb.sb("hu", [128, 128], F32); hu2 = kb.sb("hu2", [128, 128], F32); hs_ = kb.sb("hs", [128, 128], F32)
    hid = [kb.sb(f"hid{c}", [128, 128], BF16) for c in range(2)]
    w2b = kb.sb("w2b", [128, 2, 128], BF16)
    kcmpT = kb.sb("kcmpT", [128, 128], BF16); vcmp = kb.sb("vcmp", [128, 128], BF16)
    kb.memset(kcmpT.ap(), 0.0); kb.memset(vcmp.ap(), 0.0)
    kb.dma("pool", w2b.ap(), w2_d.rearrange("c h e -> h c e"))
    for c in range(2):
        rb = ring[ri % 3]; ri += 1
        w1 = rb.ap().re("p (l h) -> p l h", h=128)
        kb.dma("pool", w1, w1_d[c].rearrange("l d h -> d l h"))
        kb.dma("sp", peb[0:32, :], pe_d[c])
        pp = ps[pi % 6]; pi += 1
        kb.transpose(pp[:, 0:128], peb.ap(), ident.ap())
        kb.copy(peT.ap(), pp[:, 0:32])
        pp = ps[pi % 6]; pi += 1
        for l in range(32):
            kb.mm(pp[:, 0:1], w1[:, l, :], peT[:, l:l + 1], start=(l == 0), stop=(l == 31))
        kb.copy(cst.ap(), pp[:, 0:1])
        src = kcT if c == 0 else vcT
        pp = ps[pi % 6]; pi += 1
        for l in range(32):
            kb.mm(pp[:, 0:127], w1[:, l, :], src[:, l:l + 16 * 126 + 1:16], start=(l == 0), stop=(l == 31))
        kb.act(hu[:, 0:127], pp[:, 0:127], AF.Identity, bias=cst[:, 0:1])
        kb.tt(hu2[:, 0:127], hu[:, 0:127], hu[:, 0:127], ALU.mult)
        kb.ts(hu2[:, 0:127], hu2[:, 0:127], 0.044715, ALU.mult, 1.0, ALU.add)
        kb.tt(hu2[:, 0:127], hu2[:, 0:127], hu[:, 0:127], ALU.mult)
        kb.act(hs_[:, 0:127], hu2[:, 0:127], AF.Sigmoid, scale=1.5957691216057308)
        kb.tt(hid[c][:, 0:127], hu[:, 0:127], hs_[:, 0:127], ALU.mult)
    pp = ps[pi % 6]; pi += 1
    kb.mm(pp[:, 0:127], w2b[:, 0, :], hid[0][:, 0:127], start=True, stop=True)
    kb.copy(kcmpT[:, 0:127], pp[:, 0:127])
    pp = ps[pi % 6]; pi += 1
    kb.mm(pp[0:127, 0:128], hid[1][:, 0:127], w2b[:, 1, :], start=True, stop=True)
    kb.copy(vcmp[0:127, :], pp[0:127, 0:128])

    pts = [kb.sb(f"pt{i}", [128, 512], BF16) for i in range(3)]
    rden = [xb[0][:, 0:512], xb[0][:, 512:1024]]
    gmul = [xb[1][:, 0:512], xb[1][:, 512:1024]]
    impT = kb.sb("impT", [128, S], F32); impt = kb.sb("impt", [32, 512], F32)
    kb.memset(impT.ap(), 0.0)
    ps_s = [ps[0], ps[1]]; sets = [(ps[2], ps[3]), (ps[4], ps[5])]; ps_g = ps[6]
    si = 0; di = 0; qi = 0

    def finish_branch(h, qc, br, o_ps, dn_ps, first):
        i = qi_holder[0] % 2
        rd = rden[i]; gm = gmul[i]
        tsl = slice(512 * qc, 512 * qc + 512)
        kb.ts(rd, dn_ps.ap(), 1e-30, ALU.max)
        kb.recip(rd, rd)
        r = br * 8 + h
        kb.mm(ps_g.ap(), sel24[:, 128 * r:128 * r + 128], gT[:, tsl], start=True, stop=True)
        kb.tt(gm, ps_g.ap(), rd, ALU.mult)
        if first:
            kb.tt(oT[:, h, tsl], o_ps.ap(), gm, ALU.mult)
        else:
            kb.tt(gm, o_ps.ap(), gm, ALU.mult)
            kb.tt(oT[:, h, tsl], oT[:, h, tsl], gm, ALU.add)
        return rd
    qi_holder = [0]

    for h in range(NHC):
        for qc in range(4):
            o_ps, dn_ps = sets[qi_holder[0] % 2]
            tsl = slice(512 * qc, 512 * qc + 512)
            sp_ = ps_s[si % 2]; si += 1
            pt = pts[di % 3]; di += 1
            kb.mm(sp_.ap(), kcmpT.ap(), qT[h][:, tsl], start=True, stop=False)
            kb.mm(sp_.ap(), identb.ap(), cmpneg[:, tsl], start=False, stop=True)
            kb.act(pt.ap(), sp_.ap(), AF.Exp)
            kb.mm(o_ps.ap(), vcmp.ap(), pt.ap(), start=True, stop=True)
            kb.mm(dn_ps.ap(), onesb.ap(), pt.ap(), start=True, stop=True)
            kb.mm(ps_g[0:32, :], ovb.ap(), pt.ap(), start=True, stop=True)
            rd = rden[qi_holder[0] % 2]
            kb.ts(rd, dn_ps.ap(), 1e-30, ALU.max)
            kb.recip(rd, rd)
            kb.tt(impt.ap(), ps_g[0:32, :], rd[0:32, :], ALU.mult)
            kb.tt(impT[0:32, tsl], impT[0:32, tsl], impt.ap(), ALU.add)
            finish_branch(h, qc, 0, o_ps, dn_ps, True)
            qi_holder[0] += 1

    negselT = kb.sb("negselT", [128, S], BF16)
    kb.memset(negselT.ap(), 0.0)
    sc = kb.sb("sc", [128, 32], F32); m8 = kb.sb("m8", [128, 8], F32); sl_ = kb.sb("sl", [128, 32], F32)
    for i in range(16):
        pp = ps_s[si % 2]; si += 1
        kb.transpose(pp[:, 0:128], impT[:, 128 * i:128 * i + 128], ident.ap())
        kb.tt(sc.ap(), pp[:, 0:32], addmask[:, i, :], ALU.add)
        kb.max8(m8.ap(), sc.ap())
        kb.ts(sl_.ap(), sc.ap(), m8[:, 7:8], ALU.is_ge)
        kb.ts(sl_.ap(), sl_.ap(), -1.0, ALU.add, -NEG, ALU.mult)
        pp2 = ps_s[si % 2]; si += 1
        kb.transpose(pp2[0:32, 0:128], sl_.ap(), ident.ap())
        kb.copy(negselT[0:32, 128 * i:128 * i + 128], pp2[0:32, 0:128])

    for h in range(NHC):
        for qc in range(4):
            tsl = slice(512 * qc, 512 * qc + 512)
            rope_inplace(qT[h], tsl)

    for h in range(NHC):
        for qc in range(4):
            tsl = slice(512 * qc, 512 * qc + 512)
            for br in (1, 2):
                o_ps, dn_ps = sets[qi_holder[0] % 2]
                kts = list(range(0, 4 * qc + 4)) if br == 1 else list(range(max(0, 4 * qc - 4), 4 * qc + 4))
                kT_ = ksT if br == 1 else kwT
                v_ = vs if br == 1 else vw
                def rng(kt):
                    r = kt - 4 * qc
                    if r >= 0:
                        return 128 * r, 512, ("c", 128 * r)
                    if br == 1:
                        return 0, 512, None
                    c1 = 512 + 128 * r
                    return 0, c1 + 128, ("a", c1)
                full = [kt for kt in kts if rng(kt)[0] == 0 and rng(kt)[1] == 512]
                order = full[:1] + [kt for kt in kts if kt not in full[:1]]
                assert full, (h, qc, br)
                for n, kt in enumerate(order):
                    a, b, mk = rng(kt)
                    sp_ = ps_s[si % 2]; si += 1
                    pt = pts[di % 3]; di += 1
                    ksl = slice(128 * kt, 128 * kt + 128)
                    last_mm = (br == 2 and mk is None)
                    kb.mm(sp_[:, a:b], kT_[:, ksl], qT[h][:, 512 * qc + a:512 * qc + b], start=True, stop=last_mm)
                    if br == 1:
                        kb.mm(sp_[:, a:b], Eb[:, ksl], negselT[:, 512 * qc + a:512 * qc + b], start=False, stop=(mk is None))
                    if mk is not None:
                        mc = mk[1]
                        kb.mm(sp_[:, mc:mc + 128], identb.ap(), (cneg if mk[0] == "c" else aneg).ap(), start=False, stop=True)
                    kb.act(pt[:, a:b], sp_[:, a:b], AF.Exp)
                    kb.mm(o_ps[:, a:b], v_[:, kt, :], pt[:, a:b], start=(n == 0), stop=(n == len(order) - 1))
                    kb.mm(dn_ps[:, a:b], onesb.ap(), pt[:, a:b], start=(n == 0), stop=(n == len(order) - 1))
                finish_branch(h, qc, br, o_ps, dn_ps, False)
                qi_holder[0] += 1

    woutv = wout.rearrange("(c p) d -> p c d", p=128)
    yTv = yT.ap().re("(kt p) t -> kt p t", p=128)
    ob = [xb[2][:, 0:512], xb[2][:, 512:1024], rstd[:, 0:512]]
    oi = 0
    for st in range(8):
        rb = ring[ri % 3]; ri += 1
        w = rb[:, 0:2048].re("p (c f) -> p c f", f=256)
        kb.dma("pool", w, woutv[:, :, 256 * st:256 * st + 256])
        for jj in range(2):
            dt_ = 2 * st + jj
            for qc in range(4):
                pp = ps[pi % 6]; pi += 1
                for c in range(8):
                    kb.mm(pp.ap(), w[:, c, 128 * jj:128 * jj + 128], oT[:, c, 512 * qc:512 * qc + 512],
                          start=(c == 0), stop=(c == 7))
                o = ob[oi % 3]; oi += 1
                kb.copy(o, pp.ap(), eng=("act" if oi % 2 else "dve"))
                kb.dma("sp", yTv[dt_][:, 512 * qc:512 * qc + 512], o)
    kb.finish([yT.ap()])
    return kb


def nsa_consts():
    t = np.arange(S)
    j = np.arange(128)
    cmpneg = np.where((16 * j[:, None] + 31) <= t[None, :], 0.0, NEG).astype(np.float32)
    s_ = np.arange(128)
    cneg = np.where(s_[:, None] <= s_[None, :], 0.0, NEG).astype(np.float32)
    aneg = np.where(s_[:, None] > s_[None, :], 0.0, NEG).astype(np.float32)
    E = np.zeros((128, S), np.float32); E[:32] = (t[None, :] // 64 == np.arange(32)[:, None])
    jj = np.arange(127)[:, None]; ss = np.arange(32)[None, :]
    lo = np.maximum(jj * 16, ss * 64); hi = np.minimum(jj * 16 + 32, ss * 64 + 64)
    ov = np.zeros((128, 32), np.float32); ov[:127] = np.clip(hi - lo, 0, None).astype(np.float32) / 32
    qb = t // 64; sid = np.arange(32)
    forced = (sid[None, :] == 0) | (sid[None, :] == qb[:, None])
    valid = sid[None, :] <= qb[:, None]
    am = np.where(forced, 100.0, np.where(valid, 0.0, -100.0)).astype(np.float32)
    am = am.reshape(16, 128, 32).transpose(1, 0, 2)
    sel24 = np.zeros((128, 24, 128), np.float32)
    for r in range(24):
        sel24[r, r, :] = 1.0
    pm = np.zeros((128, 32), np.float32)
    for m in range(32):
        pm[(m + 16) % 32, m] = 1.0
    inv = np.power(np.float32(500000.0), -np.arange(0, 32, 2, dtype=np.float32) / np.float32(32)).astype(np.float32)
    rc = np.zeros((32, 3), np.float32)
    rc[:, 0] = np.concatenate([inv, inv])
    sgn = np.concatenate([-np.ones(16), np.ones(16)])
    rc[:, 1] = sgn; rc[:, 2] = sgn * math.pi
    return {"ident": np.eye(128, dtype=np.float32), "cmpneg": cmpneg, "cneg": cneg, "aneg": aneg, "E": E, "ov": ov,
            "addmask": np.ascontiguousarray(am), "sel24": sel24.reshape(128, 24 * 128), "pm": pm, "ropec": rc}


def nsa_inputs(xT_b, pos_b, g, w_in, gate_b, cmp_pe, cmp_w1, cmp_w2, w_out, p, consts):
    o1 = 2048; o2 = o1 + 512; o3 = o2 + 512; o4 = o3 + 512
    a = np.arange(128)
    cols = np.concatenate([np.arange(1024 * p, 1024 * p + 1024),
                           o1 + 128 * p + a, o1 + 256 + 128 * p + a,
                           o2 + 128 * p + a, o2 + 256 + 128 * p + a,
                           o3 + 128 * p + a, o3 + 256 + 128 * p + a,
                           np.concatenate([o4 + 16 * br + 8 * p + np.arange(8) for br in range(3)])])
    gbv = np.concatenate([gate_b[br, 8 * p:8 * p + 8] for br in range(3)]).reshape(24, 1)
    d = {"xT": np.ascontiguousarray(xT_b), "g": np.ascontiguousarray(g.reshape(NKT, 128).T),
         "win": np.ascontiguousarray(w_in[:, cols]), "gb": np.ascontiguousarray(gbv.astype(np.float32)),
         "pos": np.ascontiguousarray(np.broadcast_to(pos_b[None, :].astype(np.int32), (32, S))),
         "pe": np.ascontiguousarray(cmp_pe), "w1": np.ascontiguousarray(cmp_w1), "w2": np.ascontiguousarray(cmp_w2),
         "wout": np.ascontiguousarray(w_out[1024 * p:1024 * p + 1024])}
    d.update(consts)
    return d


def _halo(aT, p):
    out = np.zeros((aT.shape[0], 1026), np.float32)
    if p == 0:
        out[:, 2:] = aT[:, 0:1024]
    else:
        out[:, :] = aT[:, 1022:2048]
    return out


def kernel(x, positions, nsa_w_in, nsa_gate_b, nsa_cmp_pe, nsa_cmp_w1, nsa_cmp_w2, nsa_w_out,
           mlstm_w_in, mlstm_gate_b, mlstm_head_norm, mlstm_w_out,
           norm_mix, norm_ffn, ffn_w_up, ffn_conv_w, ffn_conv_b, ffn_w_down, norm_final):
    f32 = lambda a: np.asarray(a, dtype=np.float32)
    x = f32(x); positions = np.asarray(positions)
    B = x.shape[0]
    xT = [np.ascontiguousarray(x[b].T) for b in range(B)]
    consts = nsa_consts()
    cores = list(range(8))
    for i in range(4):
        j = i // 2
        if i % 2 == 0:
            kbm = build_nsa()
            ims = [nsa_inputs(xT[c // 2], positions[c // 2], f32(norm_mix[i]), f32(nsa_w_in[j]), f32(nsa_gate_b[j]),
                              f32(nsa_cmp_pe[j]), f32(nsa_cmp_w1[j]), f32(nsa_cmp_w2[j]), f32(nsa_w_out[j]), c % 2, consts)
                   for c in cores]
        else:
            kbm = build_mlstm()
            ims = [mlstm_inputs(xT[c // 2], f32(norm_mix[i]), f32(mlstm_w_in[j]), f32(mlstm_gate_b[j]),
                                f32(mlstm_head_norm[j]), f32(mlstm_w_out[j]), c % 2) for c in cores]
        res = run_bass_kernel_spmd(kbm.nc, ims, core_ids=cores)
        yp = [np.asarray(r["yT"]) for r in res.results]
        del ims
        final = (i == 3)
        kbf = build_ffn(final)
        ims = [ffn_inputs(_halo(xT[c // 2], c % 2), _halo(yp[2 * (c // 2)], c % 2), _halo(yp[2 * (c // 2) + 1], c % 2),
                          f32(norm_ffn[i]), f32(norm_final), f32(ffn_conv_w[i]), f32(ffn_conv_b[i]),
                          f32(ffn_w_up[i]), f32(ffn_w_down[i])) for c in cores]
        res = run_bass_kernel_spmd(kbf.nc, ims, core_ids=cores)
        outs = [np.asarray(r["out"]) for r in res.results]
        del ims
        xT = [np.ascontiguousarray(np.concatenate([outs[2 * b], outs[2 * b + 1]], axis=1)) for b in range(B)]
    return np.ascontiguousarray(np.stack([xT[b].T for b in range(B)], axis=0)).astype(np.float32)
```
